# Optimizing a Trainium2 kernel written in Bass

```python
import math
import jax, jax.numpy as jnp
from jax import lax
import numpy as np

D_MODEL = 1024
BATCH = 4
SEQ = 8192
DEPTH = 2

N_MIXERS = 2
N_HEADS = 16
HEAD_DIM = D_MODEL // N_HEADS
MOBA_BLOCK = 256
MOBA_TOPK = 3
MOBA_QCHUNK = 16
FOX_QBLOCK = 128
REL_BUCKETS = 32
REL_MAX_DIST = 128
N_EXPERTS = 256
TOP_K = 8
N_GROUPS = 8
TOPK_GROUPS = 4
D_EXPERT = 256
D_SHARED = 256
ROUTED_SCALE = 2.5
EXPERT_BLOCK = 128
LN_EPS = 1e-5
DN_ALPHA = (2 * DEPTH) ** 0.25
DN_BETA = (8 * DEPTH) ** -0.25
N_MOBA = (DEPTH + 1) // 2
N_FOX = DEPTH // 2
NEG = -1e30

kernel_name = "hybrid_moba_fox_moe_deepnorm"


def layer_norm(x, g, b):
    xf = x.astype(jnp.float32)
    mu = xf.mean(-1, keepdims=True)
    var = jnp.square(xf - mu).mean(-1, keepdims=True)
    return ((xf - mu) * lax.rsqrt(var + LN_EPS) * g.astype(jnp.float32) + b.astype(jnp.float32)).astype(x.dtype)


def rel_bucket(dist):
    n = jnp.maximum(dist, 0)
    max_exact = REL_BUCKETS // 2
    nf = jnp.maximum(n, 1).astype(jnp.float32)
    large = max_exact + (jnp.log(nf / max_exact) / math.log(REL_MAX_DIST / max_exact)
                         * (REL_BUCKETS - max_exact)).astype(jnp.int32)
    large = jnp.minimum(large, REL_BUCKETS - 1)
    return jnp.where(n < max_exact, n, large)


def split_heads(z):
    B, S, _ = z.shape
    return z.reshape(B, S, N_HEADS, HEAD_DIM).transpose(0, 2, 1, 3)


def merge_heads(o):
    B, H, S, dh = o.shape
    return o.transpose(0, 2, 1, 3).reshape(B, S, H * dh)


def moba_attention(q, k, v, rel_bias):
    B, H, S, dh = q.shape
    nb = -(-S // MOBA_BLOCK)
    pad = nb * MOBA_BLOCK - S
    kp = jnp.pad(k, ((0, 0), (0, 0), (0, pad), (0, 0))).reshape(B, H, nb, MOBA_BLOCK, dh)
    vp = jnp.pad(v, ((0, 0), (0, 0), (0, pad), (0, 0))).reshape(B, H, nb, MOBA_BLOCK, dh)
    k_mean = kp.astype(jnp.float32).mean(axis=3)
    n_sel = min(MOBA_TOPK, nb)
    scale = dh ** -0.5
    bias_t = rel_bias.T.astype(jnp.float32)
    bi = jnp.arange(B)[:, None, None, None]
    hi = jnp.arange(H)[None, :, None, None]
    offs = jnp.arange(MOBA_BLOCK)
    blk_ids = jnp.arange(nb)

    def chunk(c):
        t0 = c * MOBA_QCHUNK
        qc = lax.dynamic_slice_in_dim(q, t0, MOBA_QCHUNK, axis=2)
        pos = t0 + jnp.arange(MOBA_QCHUNK)
        own = t0 // MOBA_BLOCK
        bscore = jnp.einsum('bhqd,bhnd->bhqn', qc.astype(jnp.float32), k_mean)
        bscore = jnp.where(blk_ids < own, bscore, NEG)
        _, sel = lax.top_k(bscore, n_sel)
        sel_valid = sel < own
        ks = kp[bi, hi, sel]
        vs = vp[bi, hi, sel]
        s_sel = jnp.einsum('bhqd,bhqnkd->bhqnk', qc, ks, preferred_element_type=jnp.float32) * scale
        key_pos = sel[..., None] * MOBA_BLOCK + offs
        b_sel = bias_t[hi[..., None], rel_bucket(pos[:, None, None] - key_pos)]
        s_sel = jnp.where(sel_valid[..., None], s_sel + b_sel, NEG)
        s_sel = s_sel.reshape(B, H, MOBA_QCHUNK, n_sel * MOBA_BLOCK)
        k_own = lax.dynamic_index_in_dim(kp, own, axis=2, keepdims=False)
        v_own = lax.dynamic_index_in_dim(vp, own, axis=2, keepdims=False)
        own_pos = own * MOBA_BLOCK + offs
        s_own = (jnp.einsum('bhqd,bhkd->bhqk', qc, k_own, preferred_element_type=jnp.float32) * scale
                 + bias_t[:, rel_bucket(pos[:, None] - own_pos[None, :])])
        s_own = jnp.where(own_pos[None, :] <= pos[:, None], s_own, NEG)
        p = jax.nn.softmax(jnp.concatenate([s_sel, s_own], axis=-1), axis=-1)
        p_sel = p[..., :n_sel * MOBA_BLOCK].reshape(B, H, MOBA_QCHUNK, n_sel, MOBA_BLOCK)
        p_own = p[..., n_sel * MOBA_BLOCK:]
        o = (jnp.einsum('bhqnk,bhqnkd->bhqd', p_sel, vs.astype(jnp.float32))
             + jnp.einsum('bhqk,bhkd->bhqd', p_own, v_own.astype(jnp.float32)))
        return o.astype(q.dtype)

    out = lax.map(chunk, jnp.arange(S // MOBA_QCHUNK))
    return out.transpose(1, 2, 0, 3, 4).reshape(B, H, S, dh)


def forgetting_attention(q, k, v, log_f):
    B, H, S, dh = q.shape
    c = lax.cumsum(log_f, axis=2)
    scale = dh ** -0.5
    key_pos = jnp.arange(S)

    def block(i):
        t0 = i * FOX_QBLOCK
        qb = lax.dynamic_slice_in_dim(q, t0, FOX_QBLOCK, axis=2)
        cb = lax.dynamic_slice_in_dim(c, t0, FOX_QBLOCK, axis=2)
        pos = t0 + jnp.arange(FOX_QBLOCK)
        s = (jnp.einsum('bhqd,bhkd->bhqk', qb, k, preferred_element_type=jnp.float32) * scale
             + cb[..., None] - c[:, :, None, :])
        s = jnp.where(key_pos[None, :] <= pos[:, None], s, NEG)
        p = jax.nn.softmax(s, axis=-1)
        return jnp.einsum('bhqk,bhkd->bhqd', p, v.astype(jnp.float32)).astype(q.dtype)

    out = lax.map(block, jnp.arange(S // FOX_QBLOCK))
    return out.transpose(1, 2, 0, 3, 4).reshape(B, H, S, dh)


def moba_mixer(x, w_in, w_out, rel_bias):
    q, k, v = jnp.split(x @ w_in, 3, axis=-1)
    o = moba_attention(split_heads(q), split_heads(k), split_heads(v), rel_bias)
    return merge_heads(o) @ w_out


def fox_mixer(x, w_in, b_f, w_out):
    proj = x @ w_in
    D = D_MODEL
    q, k, v = proj[..., :D], proj[..., D:2 * D], proj[..., 2 * D:3 * D]
    log_f = jax.nn.log_sigmoid((proj[..., 3 * D:] + b_f).astype(jnp.float32)).transpose(0, 2, 1)
    o = forgetting_attention(split_heads(q), split_heads(k), split_heads(v), log_f)
    return merge_heads(o) @ w_out


def swiglu(h, wg, wu, wd):
    return (jax.nn.silu(h @ wg) * (h @ wu)) @ wd


def route(h, w_router, router_bias):
    T = h.shape[0]
    s = jax.nn.sigmoid((h @ w_router).astype(jnp.float32))
    sb = s + router_bias.astype(jnp.float32)
    grp = sb.reshape(T, N_GROUPS, N_EXPERTS // N_GROUPS)
    gscore = lax.top_k(grp, 2)[0].sum(-1)
    _, gidx = lax.top_k(gscore, TOPK_GROUPS)
    gmask = jax.nn.one_hot(gidx, N_GROUPS, dtype=jnp.float32).sum(-2) > 0
    emask = jnp.repeat(gmask, N_EXPERTS // N_GROUPS, axis=-1)
    _, idx = lax.top_k(jnp.where(emask, sb, NEG), TOP_K)
    w = jnp.take_along_axis(s, idx, axis=-1)
    gates = w / w.sum(-1, keepdims=True) * ROUTED_SCALE
    return idx, gates


def moe_routed(h, idx, gates, w_gate, w_up, w_down):
    T, D = h.shape
    E = w_gate.shape[0]
    flat_e = idx.reshape(-1)
    flat_t = jnp.repeat(jnp.arange(T, dtype=jnp.int32), TOP_K)
    flat_g = gates.reshape(-1)
    order = jnp.argsort(flat_e)
    se = flat_e[order]
    counts = jnp.bincount(flat_e, length=E)
    padded = (counts + EXPERT_BLOCK - 1) // EXPERT_BLOCK * EXPERT_BLOCK
    pad_end = jnp.cumsum(padded)
    pad_start = pad_end - padded
    start = jnp.cumsum(counts) - counts
    dest = pad_start[se] + jnp.arange(T * TOP_K) - start[se]
    n_blocks = (T * TOP_K + E * (EXPERT_BLOCK - 1)) // EXPERT_BLOCK
    n_pad = n_blocks * EXPERT_BLOCK
    tok = jnp.full((n_pad,), T, jnp.int32).at[dest].set(flat_t[order])
    gbuf = jnp.zeros((n_pad,), jnp.float32).at[dest].set(flat_g[order])
    blk_e = jnp.minimum(jnp.searchsorted(pad_end, jnp.arange(n_blocks) * EXPERT_BLOCK, side='right'), E - 1)
    h_pad = jnp.concatenate([h, jnp.zeros((1, D), h.dtype)], axis=0)

    def run(args):
        tb, gb, e = args
        yb = swiglu(h_pad[tb], w_gate[e], w_up[e], w_down[e])
        return yb * gb[:, None].astype(yb.dtype)

    y = lax.map(run, (tok.reshape(n_blocks, EXPERT_BLOCK), gbuf.reshape(n_blocks, EXPERT_BLOCK), blk_e))
    return jax.ops.segment_sum(y.reshape(n_pad, D), tok, num_segments=T + 1)[:T]


def moe_layer(x, w_router, router_bias, w_gate, w_up, w_down, ws_gate, ws_up, ws_down):
    B, S, D = x.shape
    h = x.reshape(B * S, D)
    idx, gates = route(h, w_router, router_bias)
    y = moe_routed(h, idx, gates, w_gate, w_up, w_down) + swiglu(h, ws_gate, ws_up, ws_down)
    return y.reshape(B, S, D)


def setup_inputs(seed: int = 0) -> dict:
    key = jax.random.key(seed)
    ks = jax.random.split(key, 20)
    D, H, E = D_MODEL, N_HEADS, N_EXPERTS
    nrm = jax.random.normal
    s_in = D ** -0.5
    qkv_scale = jnp.concatenate([jnp.full((2 * D,), s_in), jnp.full((D,), s_in * DN_BETA)])
    moba_w_in = nrm(ks[1], (N_MOBA, D, 3 * D), jnp.float32) * qkv_scale
    fox_scale = jnp.concatenate([qkv_scale, jnp.full((H,), s_in)])
    fox_w_in = nrm(ks[2], (N_FOX, D, 3 * D + H), jnp.float32) * fox_scale
    return {
        "x": nrm(ks[0], (BATCH, SEQ, D), jnp.float32),
        "rel_bias": nrm(ks[3], (REL_BUCKETS, H), jnp.float32) * 0.5,
        "moba_w_in": moba_w_in,
        "moba_w_out": nrm(ks[4], (N_MOBA, D, D), jnp.float32) * s_in * DN_BETA,
        "fox_w_in": fox_w_in,
        "fox_b_f": jax.random.uniform(ks[5], (N_FOX, H), jnp.float32, 1.0, 6.0),
        "fox_w_out": nrm(ks[6], (N_FOX, D, D), jnp.float32) * s_in * DN_BETA,
        "ln1_g": 1.0 + 0.05 * nrm(ks[7], (DEPTH, D), jnp.float32),
        "ln1_b": 0.02 * nrm(ks[8], (DEPTH, D), jnp.float32),
        "ln2_g": 1.0 + 0.05 * nrm(ks[9], (DEPTH, D), jnp.float32),
        "ln2_b": 0.02 * nrm(ks[10], (DEPTH, D), jnp.float32),
        "w_router": nrm(ks[11], (DEPTH, D, E), jnp.float32) * s_in,
        "router_bias": 0.01 * nrm(ks[12], (DEPTH, E), jnp.float32),
        "w_gate": nrm(ks[13], (DEPTH, E, D, D_EXPERT), jnp.float32) * s_in,
        "w_up": nrm(ks[14], (DEPTH, E, D, D_EXPERT), jnp.float32) * s_in * DN_BETA,
        "w_down": nrm(ks[15], (DEPTH, E, D_EXPERT, D), jnp.float32) * D_EXPERT ** -0.5 * DN_BETA,
        "ws_gate": nrm(ks[16], (DEPTH, D, D_SHARED), jnp.float32) * s_in,
        "ws_up": nrm(ks[17], (DEPTH, D, D_SHARED), jnp.float32) * s_in * DN_BETA,
        "ws_down": nrm(ks[18], (DEPTH, D_SHARED, D), jnp.float32) * D_SHARED ** -0.5 * DN_BETA,
    }


def reference(x, rel_bias, moba_w_in, moba_w_out, fox_w_in, fox_b_f, fox_w_out,
              ln1_g, ln1_b, ln2_g, ln2_b, w_router, router_bias,
              w_gate, w_up, w_down, ws_gate, ws_up, ws_down):
    for i in range(DEPTH):
        j = i // N_MIXERS
        if i % N_MIXERS == 0:
            y = moba_mixer(x, moba_w_in[j], moba_w_out[j], rel_bias)
        else:
            y = fox_mixer(x, fox_w_in[j], fox_b_f[j], fox_w_out[j])
        x = layer_norm(DN_ALPHA * x + y, ln1_g[i], ln1_b[i])
        y = moe_layer(x, w_router[i], router_bias[i], w_gate[i], w_up[i], w_down[i],
                      ws_gate[i], ws_up[i], ws_down[i])
        x = layer_norm(DN_ALPHA * x + y, ln2_g[i], ln2_b[i])
    return x
```

```python
import math
from contextlib import ExitStack

import numpy as np
import ml_dtypes

import concourse.bass as bass
import concourse.mybir as mybir
from concourse.bass_utils import run_bass_kernel_spmd

F32 = mybir.dt.float32
BF16 = mybir.dt.bfloat16
I32 = mybir.dt.int32
AF = mybir.ActivationFunctionType
ALU = mybir.AluOpType
AX = mybir.AxisListType

D = 1024
KC = 8
H = 16
DH = 64
NE = 256
DE = 256
CAP = 256
TOPK = 8
DEPTH = 2
DN_ALPHA = (2 * DEPTH) ** 0.25
LN_EPS = 1e-5
BIG = 30000.0
GL = 511
VW = 80
DBG = False
STOP_AFTER = None
EARLY = ("A1", "A2a", "A2b", "A2", "A3")


class Sched:
    CH = 30000
    NDMA = {"sp": 12, "pool": 8, "act": 4}
    NCH = {"pe": 4, "act": 3, "dve": 4, "pool": 2, "sp": 1}

    def __init__(self, nc, stack, need):
        self.nc = nc
        self.dry = need is None
        self.need = need if need is not None else set()
        self.lw = {}
        self.rd = {}
        self.n = 0
        self.sig = {}
        self.openg = {}
        self.cnt = {}
        self.waited = {e: {} for e in self.NCH}
        self.eng = {"sp": nc.sync, "pe": nc.tensor, "act": nc.scalar, "dve": nc.vector, "pool": nc.gpsimd}
        self.csem = {}
        self.dsem = {}
        self.dstate = {}
        if not self.dry:
            for e, k in self.NCH.items():
                for j in range(k):
                    self.csem[(e, j)] = stack.enter_context(nc.semaphore("c_%s_%d" % (e, j)))
            for e, k in self.NDMA.items():
                self.dsem[e] = [stack.enter_context(nc.semaphore("d_%s_%d" % (e, j))) for j in range(k)]
        for e, k in self.NDMA.items():
            self.dstate[e] = [0, [0] * k]

    def barrier(self, fn):
        self.add("dve", fn, r=(), w=["PHASE"], _bar=True)

    def add(self, eng, fn, r=(), w=(), dma=False, _bar=False):
        i = self.n
        self.n += 1
        deps = set()
        if not _bar:
            r = list(r) + ["PHASE"]
        for k in r:
            x = self.lw.get(k)
            if x is not None:
                deps.add(x)
        for k in w:
            x = self.lw.get(k)
            if x is not None:
                deps.add(x)
            for y in self.rd.get(k, ()):
                deps.add(y)
        for k in w:
            self.lw[k] = i
            self.rd[k] = []
        for k in r:
            self.rd.setdefault(k, []).append(i)
        deps.discard(i)
        self.openg[i] = (eng, dma)
        if self.dry:
            for d in deps:
                de, dd = self.openg[d]
                if de == "pe" and eng == "pe" and not dd and not dma:
                    continue
                self.need.add(d)
            return i
        E = self.eng[eng]
        waits = {}
        for d in deps:
            sg = self.sig.get(d)
            if sg is None:
                continue
            de, dd, s, v = sg
            if de == "pe" and eng == "pe" and not dd and not dma:
                continue
            if waits.get(id(s), (None, 0))[1] < v:
                waits[id(s)] = (s, v)
        sig = None
        if dma:
            stt = self.dstate[eng]
            j = stt[0] % self.NDMA[eng]
            stt[0] += 1
            prev = stt[1][j]
            s = self.dsem[eng][j]
            if prev > 0 and waits.get(id(s), (None, 0))[1] < prev:
                waits[id(s)] = (s, prev)
            stt[1][j] = prev + 16
            sig = (eng, True, s, prev + 16)
        elif i in self.need:
            c = self.cnt.get(eng, 0)
            self.cnt[eng] = c + 1
            sig = (eng, False, self.csem[(eng, c // self.CH)], c % self.CH + 1)
        wd = self.waited[eng]
        for sid, (s, v) in waits.items():
            if wd.get(sid, 0) < v:
                E.wait_ge(s, v)
                wd[sid] = v
        ins = fn(E)
        if sig is not None:
            ins.then_inc(sig[2], 16 if dma else 1)
            self.sig[i] = sig
        return i

    def finish(self):
        if self.dry:
            return
        E = self.eng["sp"]
        wd = self.waited["sp"]
        for e, k in self.NDMA.items():
            for j, v in enumerate(self.dstate[e][1]):
                s = self.dsem[e][j]
                if v > 0 and wd.get(id(s), 0) < v:
                    E.wait_ge(s, v)
                    wd[id(s)] = v


def build_program(S, kinds):
    _, need = _build_program(S, kinds, None)
    nc, _ = _build_program(S, kinds, need)
    return nc


def _build_program(S, kinds, need):
    NT = S // 2
    NOT = NT // 128
    NG = NOT // 4
    NST = S // 128
    NSC = S // 512
    NB = S // 256
    nc = bass.Bass("TRN2", target_bir_lowering=False)
    st = ExitStack()
    sch = Sched(nc, st, need)
    A = sch.add

    def din(name, shape, dt=F32):
        return nc.dram_tensor(name, list(shape), dt, kind="ExternalInput").ap()

    def dscr(name, shape, dt=F32):
        return nc.dram_tensor(name, list(shape), dt, kind=("ExternalOutput" if DBG else "Internal")).ap()

    def sb(name, shape, dt=F32):
        return st.enter_context(nc.sbuf_tensor(name, list(shape), dt))

    def ps(name, shape, dt=F32):
        return st.enter_context(nc.psum_tensor(name, list(shape), dt))

    c_ident = din("c_ident", [128, 128])
    c_triu = din("c_triu", [128, 128])
    c_trils = din("c_trils", [128, 128])
    c_kaug = {k: din("c_kaug_" + k, [32, S], BF16) for k in set(kinds)}
    c_oh = din("c_oh", [33, GL])
    c_past = din("c_past", [1, NOT * 32])
    c_iota = din("c_iota", [1, NE])
    c_par = din("c_par", [1, 2])

    ident_f = sb("ident_f", [128, 128]); ident_b = sb("ident_b", [128, 128], BF16)
    triu_f = sb("triu_f", [128, 128]); trils_b = sb("trils_b", [128, 128], BF16)
    ones_f = sb("ones_f", [128, 128]); ones_b = sb("ones_b", [128, 128], BF16)
    oh_f = sb("oh_f", [33, GL])
    past_f = sb("past_f", [128, NOT * 32])
    iota_f = sb("iota_f", [128, NE])
    par_f = sb("par_f", [128, 2])
    A("sp", lambda e: e.dma_start(out=ident_f[:], in_=c_ident[:, :]), w=["ident_f"], dma=True)
    A("sp", lambda e: e.dma_start(out=triu_f[:], in_=c_triu[:, :]), w=["triu_f"], dma=True)
    A("pool", lambda e: e.dma_start(out=trils_b[:], in_=c_trils[:, :]), w=["trils_b"], dma=True)
    A("pool", lambda e: e.dma_start(out=ident_b[:], in_=c_ident[:, :]), w=["ident_b"], dma=True)
    A("sp", lambda e: e.dma_start(out=oh_f[:], in_=c_oh[:, :]), w=["oh_f"], dma=True)
    A("sp", lambda e: e.dma_start(out=past_f[:], in_=c_past.partition_broadcast(128)), w=["past_f"], dma=True)
    A("sp", lambda e: e.dma_start(out=iota_f[:], in_=c_iota.partition_broadcast(128)), w=["iota_f"], dma=True)
    A("sp", lambda e: e.dma_start(out=par_f[:], in_=c_par.partition_broadcast(128)), w=["par_f"], dma=True)
    A("dve", lambda e: e.memset(ones_f[:], 1.0), w=["ones_f"])
    A("dve", lambda e: e.memset(ones_b[:], 1.0), w=["ones_b"])

    pb = [ps("pb%d" % i, [128, 512]) for i in range(7)]
    pbh = ps("pbh", [128, 1024], BF16)
    FMAX = bass.BassVectorEngine.BN_STATS_FMAX
    SD = bass.BassVectorEngine.BN_STATS_DIM
    AD = bass.BassVectorEngine.BN_AGGR_DIM
    nchunk = (D + FMAX - 1) // FMAX

    KT_d = dscr("KT_d", [H, DH, S], BF16)
    V_d = dscr("V_d", [H, 128, NST, DH], BF16)
    QT_d = dscr("QT_d", [H, DH, NT], BF16)
    QTf_d = dscr("QTf_d", [H, DH, NT])
    kmT_d = dscr("kmT_d", [H, DH, NB])
    G_d = [dscr("G_d%d" % i, [128, GL]) for i in range(2)]
    cq_d = dscr("cq_d", [H, 3, NT], BF16)
    ref_d = dscr("ref_d", [1, H * NG])
    x1_d = dscr("x1_d", [NT, D])
    Xs_d = dscr("Xs_d", [NE * CAP, D], BF16)
    Ys_d = dscr("Ys_d", [NE * CAP, D])

    outs = {}

    bar_t = sb("bar_t", [128, 1])

    def barrier():
        sch.barrier(lambda e: e.memset(bar_t[:], 0.0))

    def layer(li, kind, xT, xTo, xo, out_ap, W):
        with ExitStack() as lstack:
            def lsb(name, shape, dt=F32):
                return lstack.enter_context(nc.sbuf_tensor(name, list(shape), dt))
            barrier()
            layer_(li, kind, xT, xTo, xo, out_ap, W, lsb)
            barrier()

    def layer_(li, kind, xT, xTo, xo, out_ap, W, lsb):
        L = "L%d_" % li
        WIN = 3 * D + (H if kind == "fox" else 0)
        if kind == "fox":
            c_all = lsb(L + "c_all", [128, NST, H])
        with ExitStack() as ph:
            def sbp(name, shape, dt=F32):
                return ph.enter_context(nc.sbuf_tensor(L + name, list(shape), dt))
            wb = sbp("wb", [128, KC, 3 * D], BF16)
            A("pool", lambda e: e.dma_start(out=wb[:, :, 0:1536], in_=W["w_in"][:, 0:1536].rearrange("(k p) n -> p k n", p=128)),
              w=[L + "wb0"], dma=True)
            A("pool", lambda e: e.dma_start(out=wb[:, :, 1536:3072], in_=W["w_in"][:, 1536:3072].rearrange("(k p) n -> p k n", p=128)),
              w=[L + "wb1"], dma=True)
            WB = [L + "wb0", L + "wb1"]
            if kind == "fox":
                wf = sbp("wf", [128, KC, H])
                A("sp", lambda e: e.dma_start(out=wf[:], in_=W["w_in"][:, 3 * D:3 * D + H].rearrange("(k p) n -> p k n", p=128)),
                  w=[L + "wf"], dma=True)
                bfr = sbp("bfr", [128, H])
                A("sp", lambda e: e.dma_start(out=bfr[:], in_=W["b_f"].partition_broadcast(128)), w=[L + "bfr"], dma=True)
                carry = sbp("carry", [128, 2, H])
                A("dve", lambda e: e.memset(carry[:], 0.0), w=[L + "carry0", L + "carry1"])
            if kind == "moba":
                kms = sbp("kms", [128, H // 2, NB])
            xf = [sbp("xf%d" % i, [128, KC, 512]) for i in range(2)]
            xb = [sbp("xb%d" % i, [128, KC, 512], BF16) for i in range(2)]
            ktb = [sbp("ktb%d" % i, [128, 512], BF16) for i in range(2)]
            qtf = [sbp("qtf%d" % i, [128, 512]) for i in range(2)]
            vb = [sbp("vb%d" % i, [128, D], BF16) for i in range(2)]
            lft = [sbp("lft%d" % i, [128, H]) for i in range(2)]
            lf2 = [sbp("lf2%d" % i, [128, H]) for i in range(2)]
            nev = [0]
            for c in range(NSC):
                b = c % 2
                A("sp", lambda e, c=c, b=b: e.dma_start(out=xf[b][:], in_=xT[:, c * 512:(c + 1) * 512].rearrange("(k p) n -> p k n", p=128)),
                  w=[L + "xf%d" % b], dma=True)
                A("pool", lambda e, b=b: e.tensor_copy(out=xb[b][:], in_=xf[b][:]), r=[L + "xf%d" % b], w=[L + "xb%d" % b])
                for hp in range(H // 2):
                    pk = pb[hp % 2]
                    pkk = "pb%d" % (hp % 2)
                    for k in range(KC):
                        A("pe", lambda e, k=k, hp=hp, b=b, pk=pk: e.matmul(pk[:, :], wb[:, k, D + hp * 128:D + (hp + 1) * 128], xb[b][:, k, :],
                                                                        start=(k == 0), stop=(k == KC - 1)),
                          r=WB + [L + "xb%d" % b], w=[pkk])
                    kb = nev[0] % 2
                    nev[0] += 1
                    A("act", lambda e, pk=pk, kb=kb: e.copy(out=ktb[kb][:], in_=pk[:, :]), r=[pkk], w=[L + "ktb%d" % kb, pkk + "_ser"])
                    if kind == "moba":
                        A("dve", lambda e, pk=pk, hp=hp, c=c: e.tensor_reduce(out=kms[:, hp, 2 * c:2 * c + 2],
                                                                         in_=pk[:, :].rearrange("p (a b) -> p a b", b=256),
                                                                         axis=AX.X, op=ALU.add),
                          r=[pkk], w=[L + "kms", pkk + "_ser"])
                    for hh in range(2):
                        A("sp", lambda e, hp=hp, hh=hh, kb=kb, c=c: e.dma_start(out=KT_d[2 * hp + hh, :, c * 512:(c + 1) * 512],
                                                                         in_=ktb[kb][hh * 64:(hh + 1) * 64, :]),
                          r=[L + "ktb%d" % kb], w=[("KT_d", 2 * hp + hh, c)], dma=True)
                for t in range(4):
                    vbi = (c * 4 + t) % 2
                    for half in range(2):
                        pv = pb[2 + half]
                        pvk = "pb%d" % (2 + half)
                        for k in range(KC):
                            A("pe", lambda e, k=k, t=t, half=half, b=b, pv=pv: e.matmul(pv[:, :], xb[b][:, k, t * 128:(t + 1) * 128],
                                                                                  wb[:, k, 2 * D + half * 512:2 * D + (half + 1) * 512],
                                                                                  start=(k == 0), stop=(k == KC - 1)),
                              r=WB + [L + "xb%d" % b], w=[pvk])
                        A("act" if half == 0 else "dve",
                          (lambda e, pv=pv, vbi=vbi, half=half: e.copy(out=vb[vbi][:, half * 512:(half + 1) * 512], in_=pv[:, :])) if half == 0 else
                          (lambda e, pv=pv, vbi=vbi, half=half: e.tensor_copy(out=vb[vbi][:, half * 512:(half + 1) * 512], in_=pv[:, :])),
                          r=[pvk], w=[L + "vb%d_%d" % (vbi, half)])
                    j = c * 4 + t
                    A("sp", lambda e, j=j, vbi=vbi: e.dma_start(out=V_d[:, :, j, :].rearrange("h p d -> p h d"),
                                                           in_=vb[vbi][:, :].rearrange("p (h d) -> p h d", d=DH)),
                      r=[L + "vb%d_0" % vbi, L + "vb%d_1" % vbi], w=[("V_d", j)], dma=True)
                    if kind == "fox":
                        pf = pb[4]
                        for k in range(KC):
                            A("pe", lambda e, k=k, t=t, b=b: e.matmul(pf[:, 0:H], xf[b][:, k, t * 128:(t + 1) * 128], wf[:, k, :],
                                                                  start=(k == 0), stop=(k == KC - 1)),
                              r=[L + "wf", L + "xf%d" % b], w=["pb4"])
                        li2 = j % 2
                        A("dve", lambda e, li2=li2: e.tensor_tensor(out=lft[li2][:], in0=pf[:, 0:H], in1=bfr[:], op=ALU.add),
                          r=["pb4", L + "bfr"], w=[L + "lft%d" % li2])
                        A("act", lambda e, li2=li2: e.activation(out=lft[li2][:], in_=lft[li2][:], func=AF.Exp, scale=-1.0),
                          r=[L + "lft%d" % li2], w=[L + "lft%d" % li2])
                        A("act", lambda e, li2=li2: e.activation(out=lft[li2][:], in_=lft[li2][:], func=AF.Ln, bias=1.0, scale=1.0),
                          r=[L + "lft%d" % li2], w=[L + "lft%d" % li2])
                        A("dve", lambda e, li2=li2: e.tensor_scalar(out=lf2[li2][:], in0=lft[li2][:], scalar1=-1.0, scalar2=None, op0=ALU.mult),
                          r=[L + "lft%d" % li2], w=[L + "lf2%d" % li2])
                        A("pe", lambda e, li2=li2: e.matmul(pb[5][:, 0:H], triu_f[:], lf2[li2][:], start=True, stop=True),
                          r=["triu_f", L + "lf2%d" % li2], w=["pb5"])
                        A("pe", lambda e, li2=li2: e.matmul(pb[6][:, 0:H], ones_f[:], lf2[li2][:], start=True, stop=True),
                          r=["ones_f", L + "lf2%d" % li2], w=["pb6"])
                        cb, cn = j % 2, (j + 1) % 2
                        A("dve", lambda e, j=j, cb=cb: e.tensor_tensor(out=c_all[:, j, :], in0=pb[5][:, 0:H], in1=carry[:, cb, :], op=ALU.add),
                          r=["pb5", L + "carry%d" % cb], w=[(L + "c_all", j)])
                        A("dve", lambda e, cb=cb, cn=cn: e.tensor_tensor(out=carry[:, cn, :], in0=pb[6][:, 0:H], in1=carry[:, cb, :], op=ALU.add),
                          r=["pb6", L + "carry%d" % cb], w=[L + "carry%d" % cn])
            if kind == "moba":
                A("dve", lambda e: e.tensor_scalar(out=kms[:], in0=kms[:], scalar1=1.0 / 256.0, scalar2=None, op0=ALU.mult),
                  r=[L + "kms"], w=[L + "kms"])
                for hp in range(H // 2):
                    for hh in range(2):
                        A("sp", lambda e, hp=hp, hh=hh: e.dma_start(out=kmT_d[2 * hp + hh, :, :], in_=kms[hh * 64:(hh + 1) * 64, hp, :]),
                          r=[L + "kms"], w=[("kmT_d", 2 * hp + hh)], dma=True)
            for co in range(NT // 512):
                b = co % 2
                A("sp", lambda e, co=co, b=b: e.dma_start(out=xf[b][:], in_=xTo[:, co * 512:(co + 1) * 512].rearrange("(k p) n -> p k n", p=128)),
                  w=[L + "xf%d" % b], dma=True)
                A("pool", lambda e, b=b: e.tensor_copy(out=xb[b][:], in_=xf[b][:]), r=[L + "xf%d" % b], w=[L + "xb%d" % b])
                for hp in range(H // 2):
                    pk = pb[hp % 2]
                    pkk = "pb%d" % (hp % 2)
                    for k in range(KC):
                        A("pe", lambda e, k=k, hp=hp, b=b, pk=pk: e.matmul(pk[:, :], wb[:, k, hp * 128:(hp + 1) * 128], xb[b][:, k, :],
                                                                        start=(k == 0), stop=(k == KC - 1)),
                          r=WB + [L + "xb%d" % b], w=[pkk])
                    kb = nev[0] % 2
                    nev[0] += 1
                    A("act", lambda e, pk=pk, kb=kb: e.activation(out=ktb[kb][:], in_=pk[:, :], func=AF.Copy, scale=DH ** -0.5),
                      r=[pkk], w=[L + "ktb%d" % kb, pkk + "_ser"])
                    for hh in range(2):
                        A("sp", lambda e, hp=hp, hh=hh, kb=kb, co=co: e.dma_start(out=QT_d[2 * hp + hh, :, co * 512:(co + 1) * 512],
                                                                           in_=ktb[kb][hh * 64:(hh + 1) * 64, :]),
                          r=[L + "ktb%d" % kb], w=[("QT_d", 2 * hp + hh, co)], dma=True)
                    if kind == "moba":
                        A("dve", lambda e, pk=pk, kb=kb: e.tensor_copy(out=qtf[kb][:], in_=pk[:, :]), r=[pkk], w=[L + "qtf%d" % kb, pkk + "_ser"])
                        for hh in range(2):
                            A("sp", lambda e, hp=hp, hh=hh, kb=kb, co=co: e.dma_start(out=QTf_d[2 * hp + hh, :, co * 512:(co + 1) * 512],
                                                                               in_=qtf[kb][hh * 64:(hh + 1) * 64, :]),
                              r=[L + "qtf%d" % kb], w=[("QTf_d", 2 * hp + hh, co)], dma=True)
            if kind == "fox":
                c_own = sbp("c_own", [128, NOT, H])
                cav = c_all[:, :, :].rearrange("p (i two) h -> p i two h", two=2)
                allc = [(L + "c_all", j) for j in range(NST)]
                A("dve", lambda e: e.tensor_scalar(out=c_own[:], in0=cav[:, :, 0, :], scalar1=par_f[:, 0:1], scalar2=None, op0=ALU.mult),
                  r=allc + ["par_f"], w=[L + "c_own"])
                A("dve", lambda e: e.scalar_tensor_tensor(out=c_own[:], in0=cav[:, :, 1, :], scalar=par_f[:, 1:2], in1=c_own[:],
                                                       op0=ALU.mult, op1=ALU.add),
                  r=allc + ["par_f", L + "c_own"], w=[L + "c_own"])
                cqT = sbp("cqT", [H, NT])
                for i in range(NOT):
                    A("pe", lambda e, i=i: e.transpose(pb[0][0:H, 0:128], c_own[:, i, :], ident_f[:]),
                      r=[L + "c_own", "ident_f"], w=["pb0"])
                    A("dve", lambda e, i=i: e.tensor_copy(out=cqT[:, i * 128:(i + 1) * 128], in_=pb[0][0:H, 0:128]),
                      r=["pb0"], w=[L + "cqT"])
                refs = sbp("refs", [H, NG])
                cr = [sbp("cr%d" % i, [H, NT]) for i in range(2)]
                cbf = sbp("cbf", [H, 3, NT], BF16)
                A("dve", lambda e: e.tensor_copy(out=refs[:], in_=cqT[:, :].rearrange("h (g n) -> h g n", n=512)[:, :, 0]),
                  r=[L + "cqT"], w=[L + "refs"])
                for g in range(NG):
                    A("dve", lambda e, g=g: e.tensor_scalar(out=cr[0][:, g * 512:(g + 1) * 512], in0=cqT[:, g * 512:(g + 1) * 512],
                                                         scalar1=refs[:, g:g + 1], scalar2=None, op0=ALU.subtract),
                      r=[L + "cqT", L + "refs"], w=[L + "cr0"])
                for lvl in range(3):
                    src, dst = cr[lvl % 2], cr[(lvl + 1) % 2]
                    sk, dk = L + "cr%d" % (lvl % 2), L + "cr%d" % ((lvl + 1) % 2)
                    A("dve", lambda e, lvl=lvl, src=src: e.tensor_copy(out=cbf[:, lvl, :], in_=src[:]), r=[sk], w=[L + "cbf"])
                    if lvl < 2:
                        A("dve", lambda e, lvl=lvl, src=src, dst=dst: e.tensor_tensor(out=dst[:], in0=src[:], in1=cbf[:, lvl, :], op=ALU.subtract),
                          r=[sk, L + "cbf"], w=[dk])
                A("sp", lambda e: e.dma_start(out=cq_d[:, :, :], in_=cbf[:]), r=[L + "cbf"], w=["cq_d"], dma=True)
                A("sp", lambda e: e.dma_start(out=ref_d[0, :].rearrange("(h g) -> h g", g=NG), in_=refs[:]), r=[L + "refs"], w=["ref_d"], dma=True)

        if STOP_AFTER == "A1":
            return
        barrier()
        o_sb = lsb(L + "o_sb", [128, NOT, D], BF16)
        with ExitStack() as ph:
            def sbp(name, shape, dt=F32):
                return ph.enter_context(nc.sbuf_tensor(L + name, list(shape), dt))
            KTa = [sbp("KTa%d" % i, [96, S], BF16) for i in range(2)]
            Va = [sbp("Va%d" % i, [128, NST, VW], BF16) for i in range(2)]
            QTa = [sbp("QTa%d" % i, [96, NT], BF16) for i in range(2)]
            bT = [sbp("bT%d" % i, [128, 3, 128], BF16) for i in range(2)]
            PT = [sbp("PT%d" % i, [128, 512], BF16) for i in range(3)]
            rc = [sbp("rc%d" % i, [128, 4]) for i in range(2)]
            for i in range(2):
                A("pool", lambda e, i=i: e.dma_start(out=KTa[i][64:96, :], in_=c_kaug[kind][:, :]), w=[L + "KTaug%d" % i], dma=True)
                A("dve", lambda e, i=i: e.memset(Va[i][:, :, DH:VW], 1.0), w=[L + "Vone%d" % i])
                A("dve", lambda e, i=i: e.memset(QTa[i][64:96, :], 0.0), w=[L + "QTaug%d" % i])
            if kind == "moba":
                rb = sbp("rb", [33, H])
                A("dve", lambda e: e.memset(rb[:], 1.0), w=[L + "rb"])
                A("sp", lambda e: e.dma_start(out=rb[0:32, :], in_=W["rel_bias"][:, :]), r=[L + "rb"], w=[L + "rb"], dma=True)
                rbrep = [sbp("rbrep%d" % i, [33, 128]) for i in range(2)]
                gsb = [sbp("gsb%d" % i, [128, GL]) for i in range(2)]
                kmT = [sbp("kmT%d" % i, [DH, NB]) for i in range(2)]
                QTf = [sbp("QTf%d" % i, [DH, NT]) for i in range(2)]
                bsm = [sbp("bsm%d" % i, [128, 32]) for i in range(2)]
                m8 = [sbp("m8%d" % i, [128, 8]) for i in range(2)]
                mbt = [sbp("mbt%d" % i, [128, 128], BF16) for i in range(2)]
                for i in range(2):
                    A("dve", lambda e, i=i: e.memset(mbt[i][:], 0.0), w=[L + "mbt%d" % i])
            else:
                refrep = sbp("refrep", [128, H * NG])
                A("sp", lambda e: e.dma_start(out=refrep[:], in_=ref_d.partition_broadcast(128)), r=["ref_d"], w=[L + "refrep"], dma=True)
                bk = [sbp("bk%d" % i, [128, NG, NST]) for i in range(2)]
                rb = sbp("rb", [33, H])
                A("dve", lambda e: e.memset(rb[:], 0.0), w=[L + "rb"])
                A("dve", lambda e: e.memset(rb[32:33, :], 1.0), r=[L + "rb"], w=[L + "rb"])
                rbrep = [sbp("rbrep%d" % i, [33, 128]) for i in range(1)]
                gsb = [sbp("gsb%d" % i, [128, GL]) for i in range(1)]

            def build_bias_tiles(h, hb):
                gi = hb % len(gsb)
                A("dve", lambda e: e.tensor_scalar(out=rbrep[gi][:], in0=ones_f[0:33, :], scalar1=rb[:, h:h + 1], scalar2=None, op0=ALU.mult),
                  r=["ones_f", L + "rb"], w=[L + "rbrep%d" % gi])
                A("pe", lambda e: e.matmul(pb[6][:, 0:GL], rbrep[gi][:], oh_f[:], start=True, stop=True),
                  r=[L + "rbrep%d" % gi, "oh_f"], w=["pb6"])
                A("act", lambda e: e.copy(out=gsb[gi][:], in_=pb[6][:, 0:GL]), r=["pb6"], w=[L + "gsb%d" % gi])
                A("sp", lambda e: e.dma_start(out=G_d[gi][:, :], in_=gsb[gi][:]), r=[L + "gsb%d" % gi], w=["G_d%d" % gi], dma=True)
                for r_ in range(3):
                    src = bass.AP(tensor=G_d[gi].tensor, offset=r_ * 128 + 127, ap=[[GL - 1, 128], [1, 128]])
                    A("pool", lambda e, src=src, r_=r_: e.dma_start(out=bT[hb][:, r_, :], in_=src),
                      r=["G_d%d" % gi], w=[L + "bT%d_%d" % (hb, r_)], dma=True)

            if kind == "fox":
                build_bias_tiles(0, 0)
            sidx = [0]
            for h in range(H):
                hb = h % 2
                A("sp", lambda e, h=h, hb=hb: e.dma_start(out=KTa[hb][0:64, :], in_=KT_d[h, :, :]),
                  r=[("KT_d", h, c) for c in range(NSC)], w=[L + "KTa%d" % hb], dma=True)
                A("sp", lambda e, h=h, hb=hb: e.dma_start(out=Va[hb][:, :, 0:DH], in_=V_d[h, :, :, :]),
                  r=[("V_d", j) for j in range(NST)], w=[L + "Va%d" % hb], dma=True)
                A("sp", lambda e, h=h, hb=hb: e.dma_start(out=QTa[hb][0:64, :], in_=QT_d[h, :, :]),
                  r=[("QT_d", h, c) for c in range(NT // 512)], w=[L + "QTa%d" % hb], dma=True)
                qaug_keys = [L + "QTaug%d" % hb]
                bias_r = []
                if kind == "moba":
                    build_bias_tiles(h, hb)
                    bias_r = [L + "bT%d_%d" % (hb, r_) for r_ in range(3)]
                    A("sp", lambda e, h=h, hb=hb: e.dma_start(out=kmT[hb][:], in_=kmT_d[h, :, :]), r=[("kmT_d", h)], w=[L + "kmT%d" % hb], dma=True)
                    A("sp", lambda e, h=h, hb=hb: e.dma_start(out=QTf[hb][:], in_=QTf_d[h, :, :]),
                      r=[("QTf_d", h, c) for c in range(NT // 512)], w=[L + "QTf%d" % hb], dma=True)
                    for i in range(NOT):
                        ib = i % 2
                        A("pe", lambda e, i=i, hb=hb: e.matmul(pb[5][:, 0:NB], QTf[hb][:, i * 128:(i + 1) * 128], kmT[hb][:], start=True, stop=True),
                          r=[L + "QTf%d" % hb, L + "kmT%d" % hb], w=["pb5"])
                        if NB < 32:
                            A("dve", lambda e, ib=ib: e.memset(bsm[ib][:], -1e30), w=[L + "bsm%d" % ib])
                        A("dve", lambda e, i=i, ib=ib: e.tensor_tensor(out=bsm[ib][:, 0:NB], in0=pb[5][:, 0:NB], in1=past_f[:, i * 32:i * 32 + NB], op=ALU.add),
                          r=["pb5", "past_f"], w=[L + "bsm%d" % ib])
                        A("dve", lambda e, ib=ib: e.max(out=m8[ib][:], in_=bsm[ib][:]), r=[L + "bsm%d" % ib], w=[L + "m8%d" % ib])
                        A("dve", lambda e, ib=ib: e.tensor_scalar(out=mbt[ib][:, 64:96], in0=bsm[ib][:], scalar1=m8[ib][:, 3:4], scalar2=None, op0=ALU.is_lt),
                          r=[L + "bsm%d" % ib, L + "m8%d" % ib], w=[L + "mbt%d" % ib])
                        A("pe", lambda e, ib=ib: e.transpose(pbh[:, 0:128], mbt[ib][:], ident_b[:]), r=[L + "mbt%d" % ib, "ident_b"], w=["pbh"])
                        A("dve", lambda e, i=i, hb=hb: e.tensor_copy(out=QTa[hb][64:96, i * 128:(i + 1) * 128], in_=pbh[64:96, 0:128]),
                          r=["pbh"], w=[L + "QTaug%d" % hb])
                else:
                    A("sp", lambda e, h=h, hb=hb: e.dma_start(out=QTa[hb][64:67, :], in_=cq_d[h, :, :]), r=["cq_d", L + "QTaug%d" % hb],
                      w=[L + "QTaug%d" % hb], dma=True)
                    for g in range(NG):
                        A("dve", lambda e, h=h, hb=hb, g=g: e.tensor_scalar(out=bk[hb][:, g, :], in0=c_all[:, :, h], scalar1=-1.0,
                                                                         scalar2=refrep[:, h * NG + g:h * NG + g + 1], op0=ALU.mult, op1=ALU.add),
                          r=[(L + "c_all", j) for j in range(NST)] + [L + "refrep"], w=[L + "bk%d" % hb])
                    bias_r = [L + "bT0_%d" % r_ for r_ in range(3)]
                if STOP_AFTER == "A2a" or (STOP_AFTER == "A2b" and h >= 1):
                    continue
                bTh = bT[hb] if kind == "moba" else bT[0]
                hk = [L + "KTa%d" % hb, L + "KTaug%d" % hb, L + "QTa%d" % hb, L + "QTaug%d" % hb]
                for g in range(NG):
                    po = pb[3 + g % 2]
                    pok = "pb%d" % (3 + g % 2)
                    A("dve", lambda e, po=po: e.memset(po[:, :], 0.0), w=[pok])
                    nj = 8 * g + 8
                    for j in range(nj):
                        a_min = max(0, (j - 8 * g) // 2)
                        c0 = a_min * 128
                        si = sidx[0] % 3
                        sidx[0] += 1
                        pS = pb[si]
                        pSk = "pb%d" % si
                        btl = []
                        for a in range(a_min, 4):
                            r_ = 2 * (4 * g + a) + 1 - j
                            if 0 <= r_ <= (2 if kind == "moba" else 1):
                                btl.append((a, r_))
                        A("pe", lambda e, pS=pS, hb=hb, j=j, g=g, c0=c0, last=(len(btl) == 0): e.matmul(
                            pS[:, c0:512], KTa[hb][:, j * 128:(j + 1) * 128], QTa[hb][:, g * 512 + c0:(g + 1) * 512], start=True, stop=last),
                          r=hk, w=[pSk])
                        for bi, (a, r_) in enumerate(btl):
                            A("pe", lambda e, pS=pS, a=a, r_=r_, bTh=bTh, last=(bi == len(btl) - 1): e.matmul(
                                pS[:, a * 128:(a + 1) * 128], ident_b[:], bTh[:, r_, :], start=False, stop=last, skip_group_check=True),
                              r=bias_r + ["ident_b"], w=[pSk])
                        if kind == "fox":
                            A("act", lambda e, pS=pS, si=si, c0=c0, hb=hb, g=g, j=j: e.activation(out=PT[si][:, c0:512], in_=pS[:, c0:512], func=AF.Exp,
                                                                                          bias=bk[hb][:, g, j:j + 1], scale=1.0),
                              r=[pSk, L + "bk%d" % hb], w=[L + "PT%d" % si])
                        else:
                            A("act", lambda e, pS=pS, si=si, c0=c0: e.activation(out=PT[si][:, c0:512], in_=pS[:, c0:512], func=AF.Exp),
                              r=[pSk], w=[L + "PT%d" % si])
                        for a in range(a_min, 4):
                            A("pe", lambda e, po=po, si=si, a=a, hb=hb, j=j, nj=nj: e.matmul(po[:, a * 128:a * 128 + DH + 1], PT[si][:, a * 128:(a + 1) * 128],
                                                                                 Va[hb][:, j, 0:DH + 1], start=False, stop=(j == nj - 1), skip_group_check=True),
                              r=[L + "PT%d" % si, L + "Va%d" % hb, L + "Vone%d" % hb], w=[pok])
                    rb_ = g % 2
                    pov = po[:, :].rearrange("p (a c) -> p a c", c=128)
                    A("dve", lambda e, pov=pov, rb_=rb_: e.reciprocal(out=rc[rb_][:], in_=pov[:, :, DH]), r=[pok], w=[L + "rc%d" % rb_])
                    for a in range(4):
                        i = 4 * g + a
                        A("dve", lambda e, po=po, a=a, i=i, h=h, rb_=rb_: e.tensor_scalar(out=o_sb[:, i, h * DH:(h + 1) * DH], in0=po[:, a * 128:a * 128 + DH],
                                                                              scalar1=rc[rb_][:, a:a + 1], scalar2=None, op0=ALU.mult),
                          r=[pok, L + "rc%d" % rb_], w=[(L + "o_sb", i, h)])

        if STOP_AFTER in ("A2", "A2a", "A2b"):
            return
        barrier()
        d8 = lsb(L + "d8", [128, NOT * TOPK], I32)
        g8 = lsb(L + "g8", [128, NOT, TOPK])
        A("dve", lambda e: e.memset(g8[:], 0.0), w=[(L + "g8", i, j) for i in range(NOT) for j in range(TOPK)])
        with ExitStack() as ph:
            def sbp(name, shape, dt=F32):
                return ph.enter_context(nc.sbuf_tensor(L + name, list(shape), dt))
            wo = sbp("wo", [128, KC, D], BF16)
            A("pool", lambda e: e.dma_start(out=wo[:], in_=W["w_out"].rearrange("(k p) n -> p k n", p=128)), w=[L + "wo"], dma=True)
            wr = sbp("wr", [128, KC, NE])
            A("sp", lambda e: e.dma_start(out=wr[:], in_=W["w_router"].rearrange("(k p) n -> p k n", p=128)), w=[L + "wr"], dma=True)
            gb = {}
            for nm in ("ln1_g", "ln1_b"):
                gb[nm] = sbp(nm, [128, D])
                A("sp", lambda e, nm=nm: e.dma_start(out=gb[nm][:], in_=W[nm].partition_broadcast(128)), w=[L + nm], dma=True)
            rbias = sbp("rbias", [128, NE])
            A("sp", lambda e: e.dma_start(out=rbias[:], in_=W["router_bias"].partition_broadcast(128)), w=[L + "rbias"], dma=True)
            mcarry_ = sbp("mcarry", [128, 2, NE])
            A("dve", lambda e: e.memset(mcarry_[:], 0.0), w=[L + "mc0", L + "mc1"])
            OT = [sbp("OT%d" % i, [128, KC, 128], BF16) for i in range(2)]
            xt = [sbp("xt%d" % i, [128, D]) for i in range(2)]
            rr = [sbp("rr%d" % i, [128, D]) for i in range(2)]
            x1 = [sbp("x1%d" % i, [128, D]) for i in range(2)]
            x1b = [sbp("x1b%d" % i, [128, D], BF16) for i in range(2)]
            x1T = [sbp("x1T%d" % i, [128, KC, 128]) for i in range(2)]
            stt = [sbp("stt%d" % i, [128, nchunk, SD]) for i in range(2)]
            mv = [sbp("mv%d" % i, [128, AD]) for i in range(2)]
            rstd = [sbp("rstd%d" % i, [128, 1]) for i in range(2)]
            sg = [sbp("sg%d" % i, [128, NE]) for i in range(2)]
            sbb = [sbp("sbb%d" % i, [128, NE]) for i in range(2)]
            sbm = [sbp("sbm%d" % i, [128, NE]) for i in range(2)]
            gm8 = [sbp("gm8%d" % i, [128, 8, 8]) for i in range(2)]
            gsc = [sbp("gsc%d" % i, [128, 8]) for i in range(2)]
            t8 = [sbp("t8%d" % i, [128, 8]) for i in range(2)]
            pen = [sbp("pen%d" % i, [128, 8]) for i in range(2)]
            sel = [sbp("sel%d" % i, [128, NE]) for i in range(2)]
            selb = [sbp("selb%d" % i, [128, NE], BF16) for i in range(2)]
            wg_ = [sbp("wg%d" % i, [128, NE]) for i in range(2)]
            wsum = [sbp("wsum%d" % i, [128, 1]) for i in range(2)]
            pos = [sbp("pos%d" % i, [128, NE]) for i in range(2)]
            key1 = [sbp("key1%d" % i, [128, NE]) for i in range(2)]
            k8 = [sbp("k8%d" % i, [128, 8]) for i in range(2)]
            d8f = [sbp("d8f%d" % i, [128, 8]) for i in range(2)]
            junk = [sbp("junk%d" % i, [128, NE]) for i in range(2)]
            for i in range(NOT):
                b = i % 2
                ok = [(L + "o_sb", i, h) for h in range(H)]
                for k in range(KC):
                    A("pe", lambda e, i=i, k=k: e.transpose(pbh[:, k * 128:(k + 1) * 128], o_sb[:, i, k * 128:(k + 1) * 128], ident_b[:]),
                      r=ok + ["ident_b"], w=["pbh"])
                A("act", lambda e, b=b: e.copy(out=OT[b][:], in_=pbh[:, :].rearrange("p (k n) -> p k n", n=128)), r=["pbh"], w=[L + "OT%d" % b])
                for half in range(2):
                    for k in range(KC):
                        A("pe", lambda e, k=k, half=half, b=b: e.matmul(pb[half][:, :], OT[b][:, k, :], wo[:, k, half * 512:(half + 1) * 512],
                                                                   start=(k == 0), stop=(k == KC - 1)),
                          r=[L + "OT%d" % b, L + "wo"], w=["pb%d" % half])
                A("sp", lambda e, i=i, b=b: e.dma_start(out=xt[b][:], in_=xo[i * 128:(i + 1) * 128, :]), w=[L + "xt%d" % b], dma=True)
                for half in range(2):
                    A("dve", lambda e, half=half, b=b: e.scalar_tensor_tensor(out=rr[b][:, half * 512:(half + 1) * 512], in0=xt[b][:, half * 512:(half + 1) * 512],
                                                                         scalar=DN_ALPHA, in1=pb[half][:, :], op0=ALU.mult, op1=ALU.add),
                      r=[L + "xt%d" % b, "pb%d" % half], w=[L + "rr%d_%d" % (b, half)])
                rrk = [L + "rr%d_0" % b, L + "rr%d_1" % b]
                layer_norm(A, L, b, rr[b], rrk, x1[b], L + "x1%d" % b, stt[b], mv[b], rstd[b], gb["ln1_g"], gb["ln1_b"], L + "ln1_g", L + "ln1_b",
                           nchunk, FMAX, "a")
                x1k = L + "x1%d" % b
                A("sp", lambda e, i=i, b=b: e.dma_start(out=x1_d[i * 128:(i + 1) * 128, :], in_=x1[b][:]), r=[x1k], w=[("x1_d", i)], dma=True)
                A("pool", lambda e, b=b: e.tensor_copy(out=x1b[b][:], in_=x1[b][:]), r=[x1k], w=[L + "x1b%d" % b])
                for k in range(KC):
                    pt_ = pb[2 + k // 4]
                    A("pe", lambda e, k=k, b=b, pt_=pt_: e.transpose(pt_[:, (k % 4) * 128:(k % 4 + 1) * 128], x1[b][:, k * 128:(k + 1) * 128], ident_f[:]),
                      r=[x1k, "ident_f"], w=["pb%d" % (2 + k // 4)])
                for q in range(2):
                    A("act" if q == 0 else "dve",
                      (lambda e, q=q, b=b: e.copy(out=x1T[b][:, q * 4:(q + 1) * 4, :], in_=pb[2 + q][:, :].rearrange("p (k n) -> p k n", n=128))) if q == 0 else
                      (lambda e, q=q, b=b: e.tensor_copy(out=x1T[b][:, q * 4:(q + 1) * 4, :], in_=pb[2 + q][:, :].rearrange("p (k n) -> p k n", n=128))),
                      r=["pb%d" % (2 + q)], w=[L + "x1T%d_%d" % (b, q)])
                for k in range(KC):
                    A("pe", lambda e, k=k, b=b: e.matmul(pb[4][:, 0:NE], x1T[b][:, k, :], wr[:, k, :], start=(k == 0), stop=(k == KC - 1)),
                      r=[L + "x1T%d_0" % b, L + "x1T%d_1" % b, L + "wr"], w=["pb4"])
                A("act", lambda e, b=b: e.activation(out=sg[b][:], in_=pb[4][:, 0:NE], func=AF.Sigmoid), r=["pb4"], w=[L + "sg%d" % b])
                A("dve", lambda e, b=b: e.tensor_tensor(out=sbb[b][:], in0=sg[b][:], in1=rbias[:], op=ALU.add), r=[L + "sg%d" % b, L + "rbias"], w=[L + "sbb%d" % b])
                for g in range(8):
                    A("dve", lambda e, b=b, g=g: e.max(out=gm8[b][:, g, :], in_=sbb[b][:, g * 32:(g + 1) * 32]), r=[L + "sbb%d" % b], w=[L + "gm8%d_%d" % (b, g)])
                A("dve", lambda e, b=b: e.tensor_tensor(out=gsc[b][:], in0=gm8[b][:, :, 0], in1=gm8[b][:, :, 1], op=ALU.add),
                  r=[L + "gm8%d_%d" % (b, g) for g in range(8)], w=[L + "gsc%d" % b])
                A("dve", lambda e, b=b: e.max(out=t8[b][:], in_=gsc[b][:]), r=[L + "gsc%d" % b], w=[L + "t8%d" % b])
                A("dve", lambda e, b=b: e.tensor_scalar(out=pen[b][:], in0=gsc[b][:], scalar1=t8[b][:, 3:4], scalar2=1.0, op0=ALU.is_ge, op1=ALU.subtract),
                  r=[L + "gsc%d" % b, L + "t8%d" % b], w=[L + "pen%d" % b])
                A("dve", lambda e, b=b: e.tensor_scalar(out=pen[b][:], in0=pen[b][:], scalar1=1e30, scalar2=None, op0=ALU.mult),
                  r=[L + "pen%d" % b], w=[L + "pen%d" % b])
                for g in range(8):
                    A("dve", lambda e, b=b, g=g: e.tensor_scalar(out=sbm[b][:, g * 32:(g + 1) * 32], in0=sbb[b][:, g * 32:(g + 1) * 32],
                                                             scalar1=pen[b][:, g:g + 1], scalar2=None, op0=ALU.add),
                      r=[L + "sbb%d" % b, L + "pen%d" % b], w=[L + "sbm%d_%d" % (b, g)])
                sbmk = [L + "sbm%d_%d" % (b, g) for g in range(8)]
                A("dve", lambda e, b=b: e.max(out=t8[b][:], in_=sbm[b][:]), r=sbmk, w=[L + "t8%d" % b])
                A("dve", lambda e, b=b: e.tensor_scalar(out=sel[b][:], in0=sbm[b][:], scalar1=t8[b][:, 7:8], scalar2=None, op0=ALU.is_ge),
                  r=sbmk + [L + "t8%d" % b], w=[L + "sel%d" % b])
                A("pool", lambda e, b=b: e.tensor_copy(out=selb[b][:], in_=sel[b][:]), r=[L + "sel%d" % b], w=[L + "selb%d" % b])
                A("dve", lambda e, b=b: e.tensor_tensor(out=wg_[b][:], in0=sg[b][:], in1=sel[b][:], op=ALU.mult),
                  r=[L + "sg%d" % b, L + "sel%d" % b], w=[L + "wg%d" % b])
                A("dve", lambda e, b=b: e.tensor_reduce(out=wsum[b][:], in_=wg_[b][:], axis=AX.X, op=ALU.add), r=[L + "wg%d" % b], w=[L + "wsum%d" % b])
                A("dve", lambda e, b=b: e.reciprocal(out=wsum[b][:], in_=wsum[b][:]), r=[L + "wsum%d" % b], w=[L + "wsum%d" % b])
                A("dve", lambda e, b=b: e.tensor_scalar(out=wg_[b][:], in0=wg_[b][:], scalar1=wsum[b][:, 0:1], scalar2=2.5, op0=ALU.mult, op1=ALU.mult),
                  r=[L + "wg%d" % b, L + "wsum%d" % b], w=[L + "wg%d" % b])
                A("pe", lambda e, b=b: e.matmul(pb[5][:, 0:NE], trils_b[:], selb[b][:], start=True, stop=True), r=["trils_b", L + "selb%d" % b], w=["pb5"])
                A("pe", lambda e, b=b: e.matmul(pb[6][:, 0:NE], ones_b[:], selb[b][:], start=True, stop=True), r=["ones_b", L + "selb%d" % b], w=["pb6"])
                cb, cn = i % 2, (i + 1) % 2
                A("dve", lambda e, b=b, cb=cb: e.tensor_tensor(out=pos[b][:], in0=pb[5][:, 0:NE], in1=mcarry_[:, cb, :], op=ALU.add),
                  r=["pb5", L + "mc%d" % cb], w=[L + "pos%d" % b])
                A("dve", lambda e, cb=cb, cn=cn: e.tensor_tensor(out=mcarry_[:, cn, :], in0=pb[6][:, 0:NE], in1=mcarry_[:, cb, :], op=ALU.add),
                  r=["pb6", L + "mc%d" % cb], w=[L + "mc%d" % cn])
                A("dve", lambda e, b=b: e.tensor_scalar(out=pos[b][:], in0=pos[b][:], scalar1=float(CAP - 1), scalar2=None, op0=ALU.min),
                  r=[L + "pos%d" % b], w=[L + "pos%d" % b])
                A("dve", lambda e, b=b: e.tensor_tensor(out=key1[b][:], in0=pos[b][:], in1=iota_f[:], op=ALU.add), r=[L + "pos%d" % b, "iota_f"], w=[L + "key1%d" % b])
                A("dve", lambda e, b=b: e.tensor_tensor(out=key1[b][:], in0=key1[b][:], in1=sel[b][:], op=ALU.mult),
                  r=[L + "key1%d" % b, L + "sel%d" % b], w=[L + "key1%d" % b])
                A("dve", lambda e, b=b: e.max(out=k8[b][:], in_=key1[b][:]), r=[L + "key1%d" % b], w=[L + "k8%d" % b])
                A("dve", lambda e, b=b: e.tensor_scalar(out=d8f[b][:], in0=k8[b][:], scalar1=-1.0, scalar2=0.0, op0=ALU.add, op1=ALU.max), r=[L + "k8%d" % b], w=[L + "d8f%d" % b])
                A("dve", lambda e, b=b, i=i: e.tensor_copy(out=d8[:, i * TOPK:(i + 1) * TOPK], in_=d8f[b][:]), r=[L + "d8f%d" % b], w=[(L + "d8", i)])
                for j in range(TOPK):
                    A("dve", lambda e, b=b, i=i, j=j: e.scalar_tensor_tensor(out=junk[b][:], in0=key1[b][:], scalar=k8[b][:, j:j + 1], in1=wg_[b][:],
                                                                        op0=ALU.is_equal, op1=ALU.mult, accum_out=g8[:, i, j:j + 1]),
                      r=[L + "key1%d" % b, L + "k8%d" % b, L + "wg%d" % b], w=[L + "junk%d" % b, (L + "g8", i, j)])
                    A("pool", lambda e, b=b, i=i, j=j: e.indirect_dma_start(out=Xs_d[:, :], out_offset=bass.IndirectOffsetOnAxis(ap=d8[:, i * TOPK + j:i * TOPK + j + 1], axis=0),
                                                                       in_=x1b[b][:, :], in_offset=None),
                      r=[(L + "d8", i), L + "x1b%d" % b], w=["Xs_d"], dma=True)

        if STOP_AFTER == "A3":
            return
        barrier()
        with ExitStack() as ph:
            def sbp(name, shape, dt=F32):
                return ph.enter_context(nc.sbuf_tensor(L + name, list(shape), dt))
            NWB = 3
            wgt = [sbp("ewg%d" % i, [128, KC, DE], BF16) for i in range(NWB)]
            wut = [sbp("ewu%d" % i, [128, KC, DE], BF16) for i in range(NWB)]
            wdt = [sbp("ewd%d" % i, [128, 2, D], BF16) for i in range(NWB)]
            Xe = [sbp("Xe%d" % i, [128, 2, D], BF16) for i in range(2)]
            XeT = [sbp("XeT%d" % i, [128, KC, CAP], BF16) for i in range(2)]
            sil = [sbp("sil%d" % i, [128, CAP]) for i in range(2)]
            hT = [sbp("hT%d" % i, [128, 2, CAP], BF16) for i in range(2)]
            ysb = [sbp("ysb%d" % i, [128, D]) for i in range(2)]
            ny = [0]
            for ex in range(NE):
                wbi = ex % NWB
                b = ex % 2
                A("pool", lambda e, ex=ex, wbi=wbi: e.dma_start(out=wgt[wbi][:], in_=W["w_gate"][ex].rearrange("(k p) n -> p k n", p=128)), w=[L + "ewg%d" % wbi], dma=True)
                A("pool", lambda e, ex=ex, wbi=wbi: e.dma_start(out=wut[wbi][:], in_=W["w_up"][ex].rearrange("(k p) n -> p k n", p=128)), w=[L + "ewu%d" % wbi], dma=True)
                A("pool", lambda e, ex=ex, wbi=wbi: e.dma_start(out=wdt[wbi][:], in_=W["w_down"][ex].rearrange("(k p) n -> p k n", p=128)), w=[L + "ewd%d" % wbi], dma=True)
                A("sp", lambda e, ex=ex, b=b: e.dma_start(out=Xe[b][:], in_=Xs_d[ex * CAP:(ex + 1) * CAP, :].rearrange("(s p) n -> p s n", p=128)),
                  r=["Xs_d"], w=[L + "Xe%d" % b], dma=True)
                for s_ in range(2):
                    for k in range(KC):
                        A("pe", lambda e, s_=s_, k=k, b=b: e.transpose(pbh[:, k * 128:(k + 1) * 128], Xe[b][:, s_, k * 128:(k + 1) * 128], ident_b[:]),
                          r=[L + "Xe%d" % b, "ident_b"], w=["pbh"])
                    A("act" if s_ == 0 else "dve",
                      (lambda e, s_=s_, b=b: e.copy(out=XeT[b][:, :, s_ * 128:(s_ + 1) * 128], in_=pbh[:, :].rearrange("p (k n) -> p k n", n=128))) if s_ == 0 else
                      (lambda e, s_=s_, b=b: e.tensor_copy(out=XeT[b][:, :, s_ * 128:(s_ + 1) * 128], in_=pbh[:, :].rearrange("p (k n) -> p k n", n=128))),
                      r=["pbh"], w=[L + "XeT%d_%d" % (b, s_)])
                xk = [L + "XeT%d_0" % b, L + "XeT%d_1" % b]
                for hh in range(2):
                    for k in range(KC):
                        A("pe", lambda e, hh=hh, k=k, b=b, wbi=wbi: e.matmul(pb[0][:, 0:CAP], wgt[wbi][:, k, hh * 128:(hh + 1) * 128], XeT[b][:, k, :],
                                                                        start=(k == 0), stop=(k == KC - 1)),
                          r=xk + [L + "ewg%d" % wbi], w=["pb0"])
                    for k in range(KC):
                        A("pe", lambda e, hh=hh, k=k, b=b, wbi=wbi: e.matmul(pb[1][:, 0:CAP], wut[wbi][:, k, hh * 128:(hh + 1) * 128], XeT[b][:, k, :],
                                                                        start=(k == 0), stop=(k == KC - 1)),
                          r=xk + [L + "ewu%d" % wbi], w=["pb1"])
                    A("act", lambda e, hh=hh: e.activation(out=sil[hh][:], in_=pb[0][:, 0:CAP], func=AF.Silu), r=["pb0"], w=[L + "sil%d" % hh])
                    A("dve", lambda e, hh=hh, b=b: e.tensor_tensor(out=hT[b][:, hh, :], in0=sil[hh][:], in1=pb[1][:, 0:CAP], op=ALU.mult),
                      r=[L + "sil%d" % hh, "pb1"], w=[L + "hT%d_%d" % (b, hh)])
                hk_ = [L + "hT%d_0" % b, L + "hT%d_1" % b]
                for s_ in range(2):
                    yb = ny[0] % 2
                    ny[0] += 1
                    for fh in range(2):
                        for kk in range(2):
                            A("pe", lambda e, s_=s_, fh=fh, kk=kk, b=b, wbi=wbi: e.matmul(pb[2 + fh][:, :], hT[b][:, kk, s_ * 128:(s_ + 1) * 128],
                                                                                   wdt[wbi][:, kk, fh * 512:(fh + 1) * 512], start=(kk == 0), stop=(kk == 1)),
                              r=hk_ + [L + "ewd%d" % wbi], w=["pb%d" % (2 + fh)])
                        A("act" if fh == 0 else "dve",
                          (lambda e, fh=fh, yb=yb: e.copy(out=ysb[yb][:, fh * 512:(fh + 1) * 512], in_=pb[2 + fh][:, :])) if fh == 0 else
                          (lambda e, fh=fh, yb=yb: e.tensor_copy(out=ysb[yb][:, fh * 512:(fh + 1) * 512], in_=pb[2 + fh][:, :])),
                          r=["pb%d" % (2 + fh)], w=[L + "ysb%d_%d" % (yb, fh)])
                    A("sp", lambda e, ex=ex, s_=s_, yb=yb: e.dma_start(out=Ys_d[ex * CAP + s_ * 128:ex * CAP + (s_ + 1) * 128, :], in_=ysb[yb][:]),
                      r=[L + "ysb%d_0" % yb, L + "ysb%d_1" % yb], w=[("Ys_d", ex, s_)], dma=True)

        if STOP_AFTER == "M2":
            return
        barrier()
        with ExitStack() as ph:
            def sbp(name, shape, dt=F32):
                return ph.enter_context(nc.sbuf_tensor(L + name, list(shape), dt))
            sg_w = sbp("swg", [128, KC, DE], BF16)
            su_w = sbp("swu", [128, KC, DE], BF16)
            sd_w = sbp("swd", [128, 2, D], BF16)
            A("pool", lambda e: e.dma_start(out=sg_w[:], in_=W["ws_gate"].rearrange("(k p) n -> p k n", p=128)), w=[L + "swg"], dma=True)
            A("pool", lambda e: e.dma_start(out=su_w[:], in_=W["ws_up"].rearrange("(k p) n -> p k n", p=128)), w=[L + "swu"], dma=True)
            A("pool", lambda e: e.dma_start(out=sd_w[:], in_=W["ws_down"].rearrange("(k p) n -> p k n", p=128)), w=[L + "swd"], dma=True)
            gb2 = {}
            for nm in ("ln2_g", "ln2_b"):
                gb2[nm] = sbp(nm, [128, D])
                A("sp", lambda e, nm=nm: e.dma_start(out=gb2[nm][:], in_=W[nm].partition_broadcast(128)), w=[L + nm], dma=True)
            mx1_ = [sbp("mx1%d" % i, [128, D]) for i in range(2)]
            mx1b_ = [sbp("mx1b%d" % i, [128, D], BF16) for i in range(2)]
            xT_ = [sbp("mxT%d" % i, [128, KC, 128], BF16) for i in range(2)]
            msil_ = [sbp("msil%d" % i, [128, 128]) for i in range(2)]
            mhT_ = [sbp("mhT%d" % i, [128, 2, 128], BF16) for i in range(2)]
            acc = [sbp("acc%d" % i, [128, D]) for i in range(2)]
            yg = [sbp("yg%d" % i, [128, D]) for i in range(4)]
            ot = [sbp("ot%d" % i, [128, D]) for i in range(2)]
            mstt_ = [sbp("mstt%d" % i, [128, nchunk, SD]) for i in range(2)]
            mmv_ = [sbp("mmv%d" % i, [128, AD]) for i in range(2)]
            mrstd_ = [sbp("mrstd%d" % i, [128, 1]) for i in range(2)]
            ng = [0]
            for i in range(NOT):
                b = i % 2
                A("sp", lambda e, i=i, b=b: e.dma_start(out=mx1_[b][:], in_=x1_d[i * 128:(i + 1) * 128, :]), r=[("x1_d", i)], w=[L + "mx1%d" % b], dma=True)
                A("pool", lambda e, b=b: e.tensor_copy(out=mx1b_[b][:], in_=mx1_[b][:]), r=[L + "mx1%d" % b], w=[L + "mx1b%d" % b])
                for k in range(KC):
                    A("pe", lambda e, k=k, b=b: e.transpose(pbh[:, k * 128:(k + 1) * 128], mx1b_[b][:, k * 128:(k + 1) * 128], ident_b[:]),
                      r=[L + "mx1b%d" % b, "ident_b"], w=["pbh"])
                A("act", lambda e, b=b: e.copy(out=xT_[b][:], in_=pbh[:, :].rearrange("p (k n) -> p k n", n=128)), r=["pbh"], w=[L + "mxT%d" % b])
                for hh in range(2):
                    for k in range(KC):
                        A("pe", lambda e, hh=hh, k=k, b=b: e.matmul(pb[0][:, 0:128], sg_w[:, k, hh * 128:(hh + 1) * 128], xT_[b][:, k, :], start=(k == 0), stop=(k == KC - 1)),
                          r=[L + "mxT%d" % b, L + "swg"], w=["pb0"])
                    for k in range(KC):
                        A("pe", lambda e, hh=hh, k=k, b=b: e.matmul(pb[1][:, 0:128], su_w[:, k, hh * 128:(hh + 1) * 128], xT_[b][:, k, :], start=(k == 0), stop=(k == KC - 1)),
                          r=[L + "mxT%d" % b, L + "swu"], w=["pb1"])
                    A("act", lambda e, hh=hh: e.activation(out=msil_[hh][:], in_=pb[0][:, 0:128], func=AF.Silu), r=["pb0"], w=[L + "msil%d" % hh])
                    A("dve", lambda e, hh=hh, b=b: e.tensor_tensor(out=mhT_[b][:, hh, :], in0=msil_[hh][:], in1=pb[1][:, 0:128], op=ALU.mult),
                      r=[L + "msil%d" % hh, "pb1"], w=[L + "mhT%d_%d" % (b, hh)])
                for fh in range(2):
                    for kk in range(2):
                        A("pe", lambda e, fh=fh, kk=kk, b=b: e.matmul(pb[2 + fh][:, :], mhT_[b][:, kk, :], sd_w[:, kk, fh * 512:(fh + 1) * 512], start=(kk == 0), stop=(kk == 1)),
                          r=[L + "mhT%d_0" % b, L + "mhT%d_1" % b, L + "swd"], w=["pb%d" % (2 + fh)])
                    A("dve", lambda e, fh=fh, b=b: e.scalar_tensor_tensor(out=acc[b][:, fh * 512:(fh + 1) * 512], in0=mx1_[b][:, fh * 512:(fh + 1) * 512], scalar=DN_ALPHA,
                                                                     in1=pb[2 + fh][:, :], op0=ALU.mult, op1=ALU.add),
                      r=[L + "mx1%d" % b, "pb%d" % (2 + fh)], w=[L + "acc%d_%d" % (b, fh)])
                acck = [L + "acc%d_0" % b, L + "acc%d_1" % b]
                for j in range(TOPK):
                    gi = ng[0] % 4
                    ng[0] += 1
                    A("pool", lambda e, i=i, j=j, gi=gi: e.indirect_dma_start(out=yg[gi][:, :], out_offset=None, in_=Ys_d[:, :],
                                                                        in_offset=bass.IndirectOffsetOnAxis(ap=d8[:, i * TOPK + j:i * TOPK + j + 1], axis=0)),
                      r=[("Ys_d", ex, s_) for ex in range(NE) for s_ in range(2)] + [(L + "d8", i)], w=[L + "yg%d" % gi], dma=True)
                    A("dve", lambda e, i=i, j=j, gi=gi, b=b: e.scalar_tensor_tensor(out=acc[b][:], in0=yg[gi][:], scalar=g8[:, i, j:j + 1], in1=acc[b][:],
                                                                               op0=ALU.mult, op1=ALU.add),
                      r=[L + "yg%d" % gi, (L + "g8", i, j)] + acck, w=acck)
                layer_norm(A, L, b, acc[b], acck, ot[b], L + "ot%d" % b, mstt_[b], mmv_[b], mrstd_[b], gb2["ln2_g"], gb2["ln2_b"], L + "ln2_g", L + "ln2_b",
                           nchunk, FMAX, "m")
                A("sp", lambda e, i=i, b=b: e.dma_start(out=out_ap[i * 128:(i + 1) * 128, :], in_=ot[b][:]), r=[L + "ot%d" % b], w=[("out", li, i)], dma=True)

    def layer_norm(A, L, b, src, srck, dst, dstk, stt, mv, rstd, g_t, b_t, gk, bk_, nchunk, FMAX, tag):
        for cch in range(nchunk):
            lo, hi = cch * FMAX, min(D, (cch + 1) * FMAX)
            A("dve", lambda e, cch=cch, lo=lo, hi=hi: e.bn_stats(out=stt[:, cch, :], in_=src[:, lo:hi]), r=srck, w=[L + tag + "stt%d_%d" % (b, cch)])
        sk = [L + tag + "stt%d_%d" % (b, cch) for cch in range(nchunk)]
        A("dve", lambda e: e.bn_aggr(out=mv[:], in_=stt[:]), r=sk, w=[L + tag + "mv%d" % b])
        A("dve", lambda e: e.tensor_scalar(out=rstd[:], in0=mv[:, 1:2], scalar1=LN_EPS, scalar2=None, op0=ALU.add), r=[L + tag + "mv%d" % b], w=[L + tag + "rstd%d" % b])
        A("act", lambda e: e.sqrt(out=rstd[:], in_=rstd[:]), r=[L + tag + "rstd%d" % b], w=[L + tag + "rstd%d" % b])
        A("dve", lambda e: e.reciprocal(out=rstd[:], in_=rstd[:]), r=[L + tag + "rstd%d" % b], w=[L + tag + "rstd%d" % b])
        A("dve", lambda e: e.tensor_scalar(out=dst[:], in0=src[:], scalar1=mv[:, 0:1], scalar2=rstd[:, 0:1], op0=ALU.subtract, op1=ALU.mult),
          r=srck + [L + tag + "mv%d" % b, L + tag + "rstd%d" % b], w=[dstk])
        A("dve", lambda e: e.tensor_tensor(out=dst[:], in0=dst[:], in1=g_t[:], op=ALU.mult), r=[dstk, gk], w=[dstk])
        A("dve", lambda e: e.tensor_tensor(out=dst[:], in0=dst[:], in1=b_t[:], op=ALU.add), r=[dstk, bk_], w=[dstk])

    for li, kind in enumerate(kinds):
        P = "l%d_" % li
        WIN = 3 * D + (H if kind == "fox" else 0)
        W = {
            "w_in": din(P + "w_in", [D, WIN]), "w_out": din(P + "w_out", [D, D]),
            "ln1_g": din(P + "ln1_g", [1, D]), "ln1_b": din(P + "ln1_b", [1, D]),
            "ln2_g": din(P + "ln2_g", [1, D]), "ln2_b": din(P + "ln2_b", [1, D]),
            "w_router": din(P + "w_router", [D, NE]), "router_bias": din(P + "router_bias", [1, NE]),
            "ws_gate": din(P + "ws_gate", [D, DE]), "ws_up": din(P + "ws_up", [D, DE]), "ws_down": din(P + "ws_down", [DE, D]),
        }
        if STOP_AFTER not in EARLY:
            W["w_gate"] = din(P + "w_gate", [NE, D, DE]); W["w_up"] = din(P + "w_up", [NE, D, DE]); W["w_down"] = din(P + "w_down", [NE, DE, D])
        if kind == "fox":
            W["b_f"] = din(P + "b_f", [1, H])
        else:
            W["rel_bias"] = din(P + "rel_bias", [32, H])
        xT = din(P + "xT", [D, S])
        xTo = din(P + "xTo", [D, NT])
        xo = din(P + "xo", [NT, D])
        out_ap = nc.dram_tensor(P + "out", [NT, D], F32, kind="ExternalOutput").ap()
        layer(li, kind, xT, xTo, xo, out_ap, W)

    sch.finish()
    st.close()
    return nc, sch.need


def _rel_bucket(d):
    n = max(d, 0)
    if n < 16:
        return n
    nf = np.float32(max(n, 1))
    large = 16 + int(np.float32(np.log(nf / np.float32(16)) / np.float32(math.log(128 / 16)) * np.float32(16)))
    return min(large, 31)


def make_consts(S, p, kinds):
    NT = S // 2
    NOT = NT // 128
    c = {}
    c["c_ident"] = np.eye(128, dtype=np.float32)
    t = np.arange(128)
    c["c_triu"] = (t[:, None] <= t[None, :]).astype(np.float32)
    c["c_trils"] = (t[:, None] < t[None, :]).astype(np.float32)
    for k in set(kinds):
        ka = np.zeros((32, S), np.float32)
        if k == "moba":
            for n in range(S // 256):
                ka[n, n * 256:(n + 1) * 256] = -BIG
        else:
            ka[0:3, :] = 1.0
        c["c_kaug_" + k] = ka.astype(ml_dtypes.bfloat16)
    oh = np.zeros((33, GL), np.float32)
    for dd in range(GL):
        d = dd + (p - 1) * 128 - 127
        if d < 0:
            oh[32, dd] = -BIG
        else:
            oh[_rel_bucket(d), dd] += 1.0
            oh[31, dd] -= 1.0
    c["c_oh"] = oh
    past = np.zeros((NOT, 32), np.float32)
    for i in range(NOT):
        past[i, :] = -1e30
        past[i, :i] = 0.0
        if i < 32:
            past[i, i] = 1e30
    c["c_past"] = past.reshape(1, -1)
    c["c_iota"] = (np.arange(NE, dtype=np.float32) * CAP + 1.0).reshape(1, -1)
    c["c_par"] = np.array([[1.0 - p, float(p)]], np.float32)
    return c


def own_rows(S, p):
    NT = S // 2
    idx = (np.arange(NT // 128)[:, None] * 2 + p) * 128 + np.arange(128)[None, :]
    return idx.reshape(-1)


_PROG = {}


def run_layers(x, kinds, Ws, n_cores=8):
    B, S, _ = x.shape
    key = (S, tuple(kinds))
    if key not in _PROG:
        _PROG[key] = build_program(S, list(kinds))
    nc = _PROG[key]
    in_maps = []
    for c in range(n_cores):
        b, p = c // 2, c % 2
        m = make_consts(S, p, kinds)
        rows = own_rows(S, p)
        for li, W in enumerate(Ws):
            P = "l%d_" % li
            for k, v in W.items():
                if STOP_AFTER in EARLY and k in ("w_gate", "w_up", "w_down"):
                    continue
                m[P + k] = v
            xb = x[b]
            m[P + "xT"] = np.ascontiguousarray(xb.T)
            m[P + "xo"] = np.ascontiguousarray(xb[rows])
            m[P + "xTo"] = np.ascontiguousarray(xb[rows].T)
        in_maps.append(m)
    res = run_bass_kernel_spmd(nc, in_maps, core_ids=list(range(n_cores)))
    if DBG:
        global LAST_RES
        LAST_RES = res.results
    out = np.zeros((B, S, D), np.float32)
    for c in range(n_cores):
        b, p = c // 2, c % 2
        out[b, own_rows(S, p)] = res.results[c]["l%d_out" % (len(kinds) - 1)]
    return out


def layer_weights(inp, i):
    f = lambda a: np.ascontiguousarray(np.asarray(a, dtype=np.float32))
    W = {
        "ln1_g": f(inp["ln1_g"][i:i + 1]), "ln1_b": f(inp["ln1_b"][i:i + 1]),
        "ln2_g": f(inp["ln2_g"][i:i + 1]), "ln2_b": f(inp["ln2_b"][i:i + 1]),
        "w_router": f(inp["w_router"][i]), "router_bias": f(inp["router_bias"][i:i + 1]),
        "w_gate": f(inp["w_gate"][i]), "w_up": f(inp["w_up"][i]), "w_down": f(inp["w_down"][i]),
        "ws_gate": f(inp["ws_gate"][i]), "ws_up": f(inp["ws_up"][i]), "ws_down": f(inp["ws_down"][i]),
    }
    j = i // 2
    if i % 2 == 0:
        W["w_in"] = f(inp["moba_w_in"][j]); W["w_out"] = f(inp["moba_w_out"][j]); W["rel_bias"] = f(inp["rel_bias"])
    else:
        W["w_in"] = f(inp["fox_w_in"][j]); W["w_out"] = f(inp["fox_w_out"][j]); W["b_f"] = f(inp["fox_b_f"][j:j + 1])
    return W


def kernel(**inputs):
    x = np.asarray(inputs["x"], dtype=np.float32)
    for i in range(DEPTH):
        kind = "moba" if i % 2 == 0 else "fox"
        x = run_layers(x, [kind], [layer_weights(inputs, i)])
    return x
```
